# Optimizing a Trainium2 kernel written in Bass

```python
import jax, jax.numpy as jnp
from jax import lax
import numpy as np

D_MODEL = 1024
BATCH = 32
SEQ = 2048
DEPTH = 1

CHUNK = 64
Q_BLOCK = 128
EPS = 1e-6
MLA_HEADS = 8
Q_LORA = 384
KV_LORA = 256
QK_NOPE = 64
QK_ROPE = 32
V_DIM = 64
QK_DIM = QK_NOPE + QK_ROPE
ROPE_THETA = 10000.0
SB_HEADS = 8
SB_DIM = 64
N_EXPERTS = 32
TOP_K = 4
D_EXPERT = D_MODEL
SWIGLU_LIMIT = 7.0
SWIGLU_ALPHA = 1.702
EXPERT_BLOCK = 256
SPLIT_WIDTHS = (Q_LORA, KV_LORA, QK_ROPE, 3 * SB_HEADS * SB_DIM, D_MODEL, D_MODEL)
SPLIT_POINTS = tuple(int(v) for v in np.cumsum(SPLIT_WIDTHS)[:-1])
D_IN = int(sum(SPLIT_WIDTHS))

kernel_name = 'hybrid_mla_stickbreaking_moe_block'


def rmsnorm(x, g):
    xf = x.astype(jnp.float32)
    y = xf * lax.rsqrt(jnp.mean(xf * xf, axis=-1, keepdims=True) + EPS)
    return (y * g.astype(jnp.float32)).astype(x.dtype)


def modulate(h, shift, scale):
    return h * (1 + scale[:, None, :]) + shift[:, None, :]


def rope_tables(positions):
    inv_freq = 1.0 / (ROPE_THETA ** (jnp.arange(0, QK_ROPE, 2, dtype=jnp.float32) / QK_ROPE))
    ang = positions.astype(jnp.float32)[..., None] * inv_freq
    return jnp.cos(ang)[:, :, None, :], jnp.sin(ang)[:, :, None, :]


def apply_rope(x, cos, sin):
    xf = x.astype(jnp.float32)
    x1, x2 = xf[..., :QK_ROPE // 2], xf[..., QK_ROPE // 2:]
    return jnp.concatenate([x1 * cos - x2 * sin, x2 * cos + x1 * sin], axis=-1).astype(x.dtype)


def mla_attention(q, k, v):
    S = q.shape[2]
    scale = QK_DIM ** -0.5
    outs = []
    for i in range(S // Q_BLOCK):
        s0, s1 = i * Q_BLOCK, (i + 1) * Q_BLOCK
        sc = jnp.einsum('bhqd,bhkd->bhqk', q[:, :, s0:s1], k[:, :, :s1]).astype(jnp.float32) * scale
        q_chunk = (s0 + jnp.arange(Q_BLOCK)) // CHUNK
        k_chunk = jnp.arange(s1) // CHUNK
        mask = k_chunk[None, :] <= q_chunk[:, None]
        p = jax.nn.softmax(jnp.where(mask, sc, -jnp.inf), axis=-1)
        outs.append(jnp.einsum('bhqk,bhkd->bhqd', p.astype(v.dtype), v[:, :, :s1]))
    return jnp.concatenate(outs, axis=2)


def stick_breaking_attention(q, k, v):
    S = q.shape[2]
    scale = SB_DIM ** -0.5
    outs = []
    for i in range(S // Q_BLOCK):
        s0, s1 = i * Q_BLOCK, (i + 1) * Q_BLOCK
        z = jnp.einsum('bhqd,bhkd->bhqk', q[:, :, s0:s1], k[:, :, :s1]).astype(jnp.float32) * scale
        q_pos = s0 + jnp.arange(Q_BLOCK)
        k_pos = jnp.arange(s1)
        mask = k_pos[None, :] < q_pos[:, None]
        log_keep = jnp.where(mask, jax.nn.log_sigmoid(-z), 0.0)
        suffix = lax.cumsum(log_keep, axis=3, reverse=True)
        log_a = jax.nn.log_sigmoid(z) + suffix - log_keep
        a = jnp.where(mask, jnp.exp(log_a), 0.0)
        outs.append(jnp.einsum('bhqk,bhkd->bhqd', a.astype(v.dtype), v[:, :, :s1]))
    return jnp.concatenate(outs, axis=2)


def moe_ffn(h, w_router, b_router, w_gate_up, b_gate_up, w_down, b_down):
    B, S, D = h.shape
    T = B * S
    xf = h.reshape(T, D)
    logits = (xf @ w_router).astype(jnp.float32) + b_router.astype(jnp.float32)
    top_val, top_idx = lax.top_k(logits, TOP_K)
    top_w = jax.nn.softmax(top_val, axis=-1)
    n_assign = T * TOP_K
    flat_e = top_idx.reshape(-1).astype(jnp.int32)
    flat_tok = jnp.repeat(jnp.arange(T, dtype=jnp.int32), TOP_K)
    flat_w = top_w.reshape(-1)
    order = jnp.argsort(flat_e)
    sorted_e = flat_e[order]
    counts = jnp.bincount(flat_e, length=N_EXPERTS).astype(jnp.int32)
    padded = (counts + EXPERT_BLOCK - 1) // EXPERT_BLOCK * EXPERT_BLOCK
    start = jnp.cumsum(counts) - counts
    pend = jnp.cumsum(padded)
    pstart = pend - padded
    dest = pstart[sorted_e] + (jnp.arange(n_assign, dtype=jnp.int32) - start[sorted_e])
    P = ((n_assign + EXPERT_BLOCK - 1) // EXPERT_BLOCK) * EXPERT_BLOCK + N_EXPERTS * EXPERT_BLOCK
    NB = P // EXPERT_BLOCK
    buf_tok = jnp.full((P,), T, dtype=jnp.int32).at[dest].set(flat_tok[order])
    buf_w = jnp.zeros((P,), jnp.float32).at[dest].set(flat_w[order])
    block_e = jnp.minimum(
        jnp.searchsorted(pend, jnp.arange(NB, dtype=jnp.int32) * EXPERT_BLOCK, side='right'),
        N_EXPERTS - 1).astype(jnp.int32)
    xpad = jnp.concatenate([xf, jnp.zeros((1, D), xf.dtype)], axis=0)

    def expert_block(args):
        tok, e, wgt = args
        xb = xpad[tok]
        gu = xb @ w_gate_up[e] + b_gate_up[e]
        gate = jnp.minimum(gu[:, :D_EXPERT], SWIGLU_LIMIT)
        up = jnp.clip(gu[:, D_EXPERT:], -SWIGLU_LIMIT, SWIGLU_LIMIT)
        hid = (up + 1) * (gate * jax.nn.sigmoid(SWIGLU_ALPHA * gate))
        yb = hid @ w_down[e] + b_down[e]
        return yb.astype(jnp.float32) * wgt[:, None]

    ys = lax.map(expert_block, (buf_tok.reshape(NB, EXPERT_BLOCK), block_e,
                                buf_w.reshape(NB, EXPERT_BLOCK)))
    out = jnp.zeros((T + 1, D), jnp.float32).at[buf_tok].add(ys.reshape(P, D))
    return out[:T].reshape(B, S, D).astype(h.dtype)


def hybrid_layer(x, c, cos, sin, w_ada, b_ada, g_norm1, w_in, g_q_lat, w_uq, g_kv_lat, w_ukv,
                 g_qk_q, g_qk_k, w_o_mla, w_o_sb, w_out, g_norm2, w_router, b_router,
                 w_gate_up, b_gate_up, w_down, b_down):
    B, S, _ = x.shape
    mod = jax.nn.silu(c) @ w_ada + b_ada
    sh1, sc1, ga1, sh2, sc2, ga2 = jnp.split(mod, 6, axis=-1)

    h = modulate(rmsnorm(x, g_norm1), sh1, sc1)
    proj = h @ w_in
    q_lat, kv_lat, k_pe, sb_qkv, gate_a, gate_b = jnp.split(proj, SPLIT_POINTS, axis=-1)

    q = (rmsnorm(q_lat, g_q_lat) @ w_uq).reshape(B, S, MLA_HEADS, QK_DIM)
    kv = (rmsnorm(kv_lat, g_kv_lat) @ w_ukv).reshape(B, S, MLA_HEADS, QK_NOPE + V_DIM)
    k_nope, v = kv[..., :QK_NOPE], kv[..., QK_NOPE:]
    k = jnp.concatenate(
        [k_nope, jnp.broadcast_to(k_pe[:, :, None, :], (B, S, MLA_HEADS, QK_ROPE))], axis=-1)
    q = rmsnorm(q, g_qk_q)
    k = rmsnorm(k, g_qk_k)
    q = jnp.concatenate([q[..., :QK_NOPE], apply_rope(q[..., QK_NOPE:], cos, sin)], axis=-1)
    k = jnp.concatenate([k[..., :QK_NOPE], apply_rope(k[..., QK_NOPE:], cos, sin)], axis=-1)
    o_mla = mla_attention(q.transpose(0, 2, 1, 3), k.transpose(0, 2, 1, 3), v.transpose(0, 2, 1, 3))
    o_mla = o_mla.transpose(0, 2, 1, 3).reshape(B, S, MLA_HEADS * V_DIM)

    sb = sb_qkv.reshape(B, S, 3, SB_HEADS, SB_DIM).transpose(2, 0, 3, 1, 4)
    o_sb = stick_breaking_attention(sb[0], sb[1], sb[2])
    o_sb = o_sb.transpose(0, 2, 1, 3).reshape(B, S, SB_HEADS * SB_DIM)

    merged = jax.nn.sigmoid(gate_a) * (o_mla @ w_o_mla) + jax.nn.sigmoid(gate_b) * (o_sb @ w_o_sb)
    x = x + ga1[:, None, :] * (merged @ w_out)

    h2 = modulate(rmsnorm(x, g_norm2), sh2, sc2)
    x = x + ga2[:, None, :] * moe_ffn(h2, w_router, b_router, w_gate_up, b_gate_up, w_down, b_down)
    return x


def setup_inputs(seed: int = 0) -> dict:
    key = jax.random.key(seed)
    ks = jax.random.split(key, 26)
    f32 = jnp.float32
    L = DEPTH

    def nrm(k, shape, scale):
        return jax.random.normal(k, shape, f32) * scale

    def gain(k, shape):
        return 1.0 + 0.02 * jax.random.normal(k, shape, f32)

    x = jax.random.normal(ks[0], (BATCH, SEQ, D_MODEL), f32)
    c = jax.random.normal(ks[1], (BATCH, D_MODEL), f32)
    offset = jax.random.randint(ks[2], (BATCH, 1), 0, 8192, dtype=jnp.int32)
    positions = offset + jnp.arange(SEQ, dtype=jnp.int32)[None, :]
    return {
        'x': x,
        'c': c,
        'positions': positions,
        'w_ada': nrm(ks[3], (L, D_MODEL, 6 * D_MODEL), 0.5 * D_MODEL ** -0.5),
        'b_ada': nrm(ks[4], (L, 6 * D_MODEL), 0.02),
        'g_norm1': gain(ks[5], (L, D_MODEL)),
        'w_in': nrm(ks[6], (L, D_MODEL, D_IN), D_MODEL ** -0.5),
        'g_q_lat': gain(ks[7], (L, Q_LORA)),
        'w_uq': nrm(ks[8], (L, Q_LORA, MLA_HEADS * QK_DIM), Q_LORA ** -0.5),
        'g_kv_lat': gain(ks[9], (L, KV_LORA)),
        'w_ukv': nrm(ks[10], (L, KV_LORA, MLA_HEADS * (QK_NOPE + V_DIM)), KV_LORA ** -0.5),
        'g_qk_q': gain(ks[11], (L, QK_DIM)),
        'g_qk_k': gain(ks[12], (L, QK_DIM)),
        'w_o_mla': nrm(ks[13], (L, MLA_HEADS * V_DIM, D_MODEL), (MLA_HEADS * V_DIM) ** -0.5),
        'w_o_sb': nrm(ks[14], (L, SB_HEADS * SB_DIM, D_MODEL), (SB_HEADS * SB_DIM) ** -0.5),
        'w_out': nrm(ks[15], (L, D_MODEL, D_MODEL), D_MODEL ** -0.5),
        'g_norm2': gain(ks[16], (L, D_MODEL)),
        'w_router': nrm(ks[17], (L, D_MODEL, N_EXPERTS), D_MODEL ** -0.5),
        'b_router': nrm(ks[18], (L, N_EXPERTS), 0.01),
        'w_gate_up': nrm(ks[19], (L, N_EXPERTS, D_MODEL, 2 * D_EXPERT), D_MODEL ** -0.5),
        'b_gate_up': nrm(ks[20], (L, N_EXPERTS, 2 * D_EXPERT), 0.02),
        'w_down': nrm(ks[21], (L, N_EXPERTS, D_EXPERT, D_MODEL), D_EXPERT ** -0.5),
        'b_down': nrm(ks[22], (L, N_EXPERTS, D_MODEL), 0.02),
    }


def reference(x, c, positions, w_ada, b_ada, g_norm1, w_in, g_q_lat, w_uq, g_kv_lat, w_ukv,
              g_qk_q, g_qk_k, w_o_mla, w_o_sb, w_out, g_norm2, w_router, b_router,
              w_gate_up, b_gate_up, w_down, b_down):
    cos, sin = rope_tables(positions)
    for l in range(DEPTH):
        x = hybrid_layer(x, c, cos, sin, w_ada[l], b_ada[l], g_norm1[l], w_in[l], g_q_lat[l],
                         w_uq[l], g_kv_lat[l], w_ukv[l], g_qk_q[l], g_qk_k[l], w_o_mla[l],
                         w_o_sb[l], w_out[l], g_norm2[l], w_router[l], b_router[l],
                         w_gate_up[l], b_gate_up[l], w_down[l], b_down[l])
    return x
```

```python
import numpy as np
from contextlib import ExitStack
import concourse.bass as bass
import concourse.mybir as mybir
from concourse.bass_utils import run_bass_kernel_spmd

F32 = mybir.dt.float32
BF16 = mybir.dt.bfloat16
I32 = mybir.dt.int32
ALU = mybir.AluOpType
AF = mybir.ActivationFunctionType
AX = mybir.AxisListType

D = 1024
S = 2048
NT = 16
H = 8
DIN = 4256
NE = 32
BLK = 256
NBM = 160
EPS = 1e-6


class Sched:
    EPOCH = 20000
    NDMA = 16
    SAME_DIST = 1 << 40

    def __init__(self, nc, stack):
        self.nc = nc
        self.stack = stack
        self.eng = {'pe': nc.tensor, 'act': nc.scalar, 'dve': nc.vector,
                    'pool': nc.gpsimd, 'sp': nc.sync}
        self.n = {e: 0 for e in self.eng}
        self.esem = {}
        self.dsem = {q: [stack.enter_context(nc.semaphore(f"d_{q}_{i}")) for i in range(self.NDMA)]
                     for q in ('sp', 'pool', 'poolc')}
        self.dn = {q: 0 for q in ('sp', 'pool', 'poolc')}
        self.waited = {e: {} for e in self.eng}
        self.last_w = {}
        self.readers = {}
        self.nwaits = 0
        self.nops = 0
        self.pend = {}

    def _esem(self, e, n):
        ep = (n - 1) // self.EPOCH
        k = (e, ep)
        if k not in self.esem:
            self.esem[k] = self.stack.enter_context(self.nc.semaphore(f"c_{e}_{ep}"))
        return self.esem[k], (n - 1) % self.EPOCH + 1

    def _wait(self, e, tok):
        sem, val, src = tok[0], tok[1], tok[2]
        sid = id(sem)
        if self.waited[e].get(sid, 0) >= val:
            return
        self.waited[e][sid] = val
        self.eng[e].wait_ge(sem, val)
        self.nwaits += 1

    def _deps(self, e, reads, writes):
        toks = []
        for k in reads:
            t = self.last_w.get(k)
            if t is not None:
                toks.append(t)
        for k in writes:
            t = self.last_w.get(k)
            if t is not None:
                toks.append(t)
            toks.extend(self.readers.get(k, ()))
        for t in toks:
            if e == 'pe' and t[2] == 'pe':
                continue
            if t[2] == e and len(t) > 3 and self.n[e] - t[3] >= self.SAME_DIST:
                continue
            self._wait(e, t)

    def _commit(self, tok, reads, writes):
        for k in reads:
            self.readers.setdefault(k, []).append(tok)
        for k in writes:
            self.last_w[k] = tok
            self.readers[k] = []

    def op(self, e, fn, reads=(), writes=(), inc=True):
        self._deps(e, reads, writes)
        if not inc:
            fn(self.eng[e])
            self.nops += 1
            pr, pw = self.pend.setdefault(e, ([], []))
            pr.extend(reads)
            pw.extend(writes)
            return
        self.n[e] += 1
        sem, val = self._esem(e, self.n[e])
        ins = fn(self.eng[e])
        ins.then_inc(sem, 1)
        self.nops += 1
        pr, pw = self.pend.pop(e, ([], []))
        self._commit((sem, val, e, self.n[e]), list(reads) + pr, list(writes) + pw)

    def dma(self, q, fn, reads=(), writes=()):
        e = 'pool' if q == 'poolc' else q
        self._deps(e, reads, writes)
        i = self.dn[q]
        self.dn[q] += 1
        sem = self.dsem[q][i % self.NDMA]
        rnd = i // self.NDMA
        if rnd > 0:
            self._wait(e, (sem, 16 * rnd, 'dma'))
        ins = fn(self.eng[e])
        ins.then_inc(sem, 16)
        self.nops += 1
        self._commit((sem, 16 * (rnd + 1), 'dma'), reads, writes)

    def barrier(self):
        assert not self.pend, self.pend.keys()
        toks = []
        for e in ('pe', 'act', 'dve', 'pool'):
            if self.n[e] > 0:
                sem, val = self._esem(e, self.n[e])
                toks.append((sem, val, e))
        for q in self.dsem:
            for j, sem in enumerate(self.dsem[q]):
                cnt = (self.dn[q] - j + self.NDMA - 1) // self.NDMA
                if cnt > 0:
                    toks.append((sem, 16 * cnt, 'dma'))
        for e in self.eng:
            for t in toks:
                self._wait(e, t)
        self.last_w = {}
        self.readers = {}


C_ID, C_R, C_U, C_LO, C_ONE = 0, 128, 256, 384, 512
C_IOTA = 640
C_FREQ = 641
C_THR = 657
C_NU = 657 + NBM
C_IOE = C_NU + 128
NCONST = C_IOE + NE


def make_consts():
    c = np.zeros((128, NCONST), np.float32)
    j = np.arange(128)[:, None]
    s = np.arange(128)[None, :]
    c[:, C_ID:C_ID + 128] = (j == s)
    c[:, C_R:C_R + 128] = (j < s)
    c[:, C_U:C_U + 128] = (j > s)
    c[:, C_LO:C_LO + 128] = (j <= s)
    c[:, C_ONE:C_ONE + 128] = 1.0
    c[:, C_IOTA] = np.arange(128)
    inv = (1.0 / (np.float32(10000.0) ** (np.arange(0, 32, 2, dtype=np.float32) / np.float32(32)))).astype(np.float32)
    c[:, C_FREQ:C_FREQ + 16] = inv[None, :]
    c[:, C_THR:C_THR + NBM] = (np.arange(NBM) * BLK)[None, :]
    c[:, C_IOE:C_IOE + NE] = np.arange(NE)[None, :]
    c[:, C_NU:C_NU + 128] = -(j >= s).astype(np.float32)
    return c


class _Stop(Exception):
    pass


def build(nseq=4, dbg=False, upto=None):
    nc = bass.Bass("TRN2", target_bir_lowering=False)
    NTOK = nseq * S
    NTT = nseq * NT
    NB = (NTOK * 4) // BLK + NE
    P = NB * BLK

    def din(name, shape, dt=F32):
        return nc.dram_tensor(name, shape, dt, kind="ExternalInput").ap()

    x_d = din("x", [NTOK, D])
    cT_d = din("cT", [128, 8, nseq])
    pos_d = din("posi", [128, nseq, NT], I32)
    w_ada_d = din("w_ada", [D, 6 * D])
    b_adaT_d = din("b_adaT", [128, 48])
    gvec_d = din("gvec", [128, 21])
    gqk_d = din("gqk", [128, 2, 96])
    w_in_d = din("w_in", [D, DIN])
    w_uq_d = din("w_uq", [384, 768])
    w_ukv_d = din("w_ukv", [256, 1024])
    w_om_d = din("w_o_mla", [512, D])
    w_os_d = din("w_o_sb", [512, D])
    w_out_d = din("w_out", [D, D])
    w_r_d = din("w_router", [D, NE])
    b_r_d = din("b_router_bc", [128, NE])
    w_gu_d = din("w_gu", [NE * D, 2 * D])
    b_gu_d = din("b_guT", [128, 16, NE])
    w_d_d = din("w_d", [NE * D, D])
    b_d_d = din("b_d", [NE, D])
    consts_d = din("consts", [128, NCONST])
    out_d = nc.dram_tensor("out", [NTOK, D], F32, kind="ExternalOutput").ap()

    skind = "ExternalOutput" if dbg else "Internal"

    def dscr(name, shape, dt):
        return nc.dram_tensor(name, shape, dt, kind=skind).ap()

    QT = dscr("QT", [nseq, H, 96, S], BF16)
    KT = dscr("KT", [nseq, H, 96, S], BF16)
    VV = dscr("VV", [nseq, S, 512], BF16)
    SQT = dscr("SQT", [nseq, 512, S], BF16)
    SKT = dscr("SKT", [nseq, 512, S], BF16)
    SV = dscr("SV", [nseq, S, 512], BF16)
    GS = dscr("GS", [nseq, S, 2048], BF16)
    OM = dscr("OM", [nseq, 512, S], BF16)
    OS = dscr("OS", [nseq, 512, S], BF16)
    H2 = dscr("H2", [NTOK, D], BF16)
    XS = dscr("XS", [P, D], BF16)
    YS = dscr("YS", [P, D], F32)
    WINB = nc.dram_tensor("WINB", [D, DIN], BF16, kind="Internal").ap()
    WGB = nc.dram_tensor("WGB", [NE * 128, 8 * 2 * D], BF16, kind="Internal").ap()
    WDB = nc.dram_tensor("WDB", [NE * 128, 8 * D], BF16, kind="Internal").ap()
    if dbg:
        LGd = dscr("LGd", [128, NTT * NE], F32)
        DSTd = dscr("DSTd", [128, NTT * 4], I32)
        W4d = dscr("W4d", [128, NTT * 4], F32)
        BEd = dscr("BEd", [128, NBM], F32)

    try:
        with ExitStack() as top:
            Sc = Sched(nc, top)
            op, dma = Sc.op, Sc.dma

            def ck(name):
                if upto == name:
                    Sc.barrier()
                    raise _Stop()

            uid = [0]

            def T(st, name, shape, dt):
                uid[0] += 1
                return st.enter_context(nc.sbuf_tensor(f"s{uid[0]}_{name}", shape, dt))

            def PS(st, name, shape, dt=F32):
                uid[0] += 1
                return st.enter_context(nc.psum_tensor(f"p{uid[0]}_{name}", shape, dt))

            cst = T(top, "cst", [128, NCONST], F32)
            cbf = T(top, "cbf", [128, 640], BF16)
            nub = T(top, "nub", [128, 128], BF16)
            modT = T(top, "modT", [128, 48, nseq], F32)
            a1T = T(top, "a1T", [128, 8, nseq], F32)
            a2T = T(top, "a2T", [128, 8, nseq], F32)
            gvec = T(top, "gvec", [128, 21], F32)
            gqk = T(top, "gqk", [128, 2, 96], F32)
            posf = T(top, "posf", [128, nseq, NT], F32)
            W4 = T(top, "W4", [128, NTT, 4], F32)
            DEST = T(top, "DEST", [128, NTT, 4], I32)
            cum = T(top, "cum", [128, NE], F32)
            brbc = T(top, "brbc", [128, NE], F32)
            bc = T(top, "bc", [128, 3, D], F32)
            rep = T(top, "rep", [128, 2, 128], F32)
            BE = T(top, "BE", [128, NBM], F32)
            mid = ExitStack()
            LG = T(mid, "LG", [128, NTT, NE], F32)
            V8 = T(mid, "V8", [128, NTT, 8], F32)
            POS = T(mid, "POS", [128, NTT, NE], F32)

            ident = cst[:, C_ID:C_ID + 128]
            identb = cbf[:, C_ID:C_ID + 128]
            Rb = cbf[:, C_R:C_R + 128]
            Ub = cbf[:, C_U:C_U + 128]
            Lob = cbf[:, C_LO:C_LO + 128]
            onesb = cbf[:, C_ONE:C_ONE + 128]
            Rf = cst[:, C_R:C_R + 128]

            dma('sp', lambda e: e.dma_start(out=cst[:], in_=consts_d), writes=['cst'])
            dma('sp', lambda e: e.dma_start(out=gvec[:], in_=gvec_d), writes=['gvec'])
            dma('sp', lambda e: e.dma_start(out=gqk[:], in_=gqk_d), writes=['gqk'])
            dma('sp', lambda e: e.dma_start(out=brbc[:], in_=b_r_d), writes=['brbc'])
            op('dve', lambda e: e.tensor_copy(out=cbf[:], in_=cst[:, 0:640]), reads=['cst'], writes=['cbf'])
            op('dve', lambda e: e.tensor_copy(out=nub[:], in_=cst[:, C_NU:C_NU + 128]), reads=['cst'], writes=['cbf'])
            op('dve', lambda e: e.memset(cum[:], 0.0), writes=['cum'])

            with ExitStack() as st:
                cT = T(st, "cTs", [128, 8, nseq], F32)
                scT = T(st, "scT", [128, 8, nseq], F32)
                badaT = T(st, "badaT", [128, 48], F32)
                posi = T(st, "posi", [128, nseq, NT], I32)
                wa = [T(st, f"wa{i}", [128, 6 * D], F32) for i in range(2)]
                pmod = PS(st, "pmod", [128, 48 * nseq])
                zt = T(st, "zt", [128, 8192], BF16)
                op('pool', lambda e: e.memset(zt[:], 0.0), writes=['zt'])
                rows_per = 128 * 8
                nz = P // rows_per
                for zi in range(nz):
                    dma('sp', lambda e: e.dma_start(out=XS[zi * rows_per:(zi + 1) * rows_per, :].rearrange("(p r) d -> p (r d)", p=128), in_=zt[:]),
                        reads=['zt'], writes=[('XSz', zi)])
                for kc in range(8):
                    for (ca, cb_) in ((0, 2048), (2048, 4096), (4096, DIN)):
                        dma('poolc', lambda e: e.dma_start(out=WINB[kc * 128:(kc + 1) * 128, ca:cb_], in_=w_in_d[kc * 128:(kc + 1) * 128, ca:cb_]),
                            writes=[('WINB', kc, ca)])
                dma('sp', lambda e: e.dma_start(out=cT[:], in_=cT_d), writes=['cT'])
                dma('sp', lambda e: e.dma_start(out=badaT[:], in_=b_adaT_d), writes=['badaT'])
                dma('sp', lambda e: e.dma_start(out=posi[:], in_=pos_d), writes=['posi'])
                op('dve', lambda e: e.tensor_copy(out=posf[:], in_=posi[:]), reads=['posi'], writes=['posf'])
                op('act', lambda e: e.activation(out=scT[:], in_=cT[:], func=AF.Silu), reads=['cT'], writes=['scT'])
                for k in range(8):
                    w = wa[k % 2]
                    wk = f'wa{k % 2}'
                    dma('sp', lambda e: e.dma_start(out=w[:], in_=w_ada_d[k * 128:(k + 1) * 128, :]), writes=[wk])
                    for j in range(48):
                        op('pe', lambda e: e.matmul(pmod[:, j * nseq:(j + 1) * nseq], lhsT=w[:, j * 128:(j + 1) * 128],
                                                    rhs=scT[:, k, :], start=(k == 0 and j == 0), stop=(k == 7),
                                                    skip_group_check=True),
                           reads=[wk, 'scT'], writes=['pmod'])
                op('dve', lambda e: e.tensor_tensor(out=modT[:], in0=pmod[:].rearrange("p (j b) -> p j b", b=nseq),
                                                    in1=badaT[:].unsqueeze(2).to_broadcast([128, 48, nseq]), op=ALU.add),
                   reads=['pmod', 'badaT'], writes=['modT'])
                for (aT, gofs, mofs, nm) in ((a1T, 0, 8, 'a1T'), (a2T, 8, 32, 'a2T')):
                    op('dve', lambda e: e.scalar_tensor_tensor(out=aT[:], in0=modT[:, mofs:mofs + 8, :], scalar=1.0,
                                                               in1=gvec[:, gofs:gofs + 8].unsqueeze(2).to_broadcast([128, 8, nseq]),
                                                               op0=ALU.add, op1=ALU.mult),
                       reads=['modT', 'gvec'], writes=[nm])
                Sc.barrier()
            if upto == 'P0':
                return nc

            pbc_stack = ExitStack()

            def make_bc(slot, vec_fn, key, pbc):
                for c in range(8):
                    r = rep[:, c % 2, :]
                    op('dve', lambda e: e.tensor_copy(out=r, in_=vec_fn(c).to_broadcast([128, 128])),
                       reads=[key], writes=[f'rep{c % 2}'])
                    op('pe', lambda e: e.matmul(pbc[:, c * 128:(c + 1) * 128], lhsT=r, rhs=ident, start=True, stop=True,
                                                skip_group_check=True),
                       reads=[f'rep{c % 2}', 'cst'], writes=['pbc'])
                op('act', lambda e: e.activation(out=bc[:, slot, :], in_=pbc[:], func=AF.Copy), reads=['pbc'], writes=[f'bc{slot}'])

            def rstd_chain(v, key, n_scale, eps=EPS):
                op('dve', lambda e: e.tensor_scalar(out=v, in0=v, scalar1=n_scale, scalar2=eps, op0=ALU.mult, op1=ALU.add),
                   reads=[key], writes=[key])
                op('act', lambda e: e.activation(out=v, in_=v, func=AF.Ln), reads=[key], writes=[key])
                op('act', lambda e: e.activation(out=v, in_=v, func=AF.Exp, scale=-0.5), reads=[key], writes=[key])

            def conv_jobs():
                for ei in range(NE):
                    for kh in range(2):
                        yield lambda: dma('poolc', lambda e: e.dma_start(
                            out=WGB[ei * 128:(ei + 1) * 128, :].rearrange("p (kc c) -> p kc c", kc=8)[:, kh * 4:(kh + 1) * 4, :],
                            in_=w_gu_d[ei * D + kh * 512:ei * D + (kh + 1) * 512, :].rearrange("(kc p) c -> p kc c", p=128)), writes=[('WGB', ei, kh)])
                    yield lambda: dma('poolc', lambda e: e.dma_start(
                        out=WDB[ei * 128:(ei + 1) * 128, :].rearrange("p (kc c) -> p kc c", kc=8),
                        in_=w_d_d[ei * D:(ei + 1) * D, :].rearrange("(kc p) c -> p kc c", p=128)), writes=[('WDB', ei)])
            conv = conv_jobs()

            def conv_step(k):
                for _ in range(k):
                    j = next(conv, None)
                    if j is not None:
                        j()

            for b in range(nseq):
                with ExitStack() as st:
                    st.enter_context(nc.named_scope(f'P1_{b}'))
                    win = T(st, "win", [128, 8, DIN], BF16)
                    wuq = T(st, "wuq", [128, 3, 768], BF16)
                    wukv = T(st, "wukv", [128, 2, 1024], BF16)
                    wst = T(st, "wst", [128, 1024], F32)
                    cb = T(st, "cb", [1, DIN], BF16)
                    shb = T(st, "shb", [128, 8], BF16)
                    xt = [T(st, f"xt{i}", [128, D], F32) for i in range(2)]
                    junk = T(st, "junk", [128, D], BF16)
                    hb = [T(st, f"hb{i}", [128, D], BF16) for i in range(2)]
                    hT = [T(st, f"hT{i}", [128, 8, 128], BF16) for i in range(2)]
                    ss = T(st, "ss", [128, 4], F32)
                    lat = T(st, "lat", [128, 672], F32)
                    latb = T(st, "latb", [128, 640], BF16)
                    latT = T(st, "latT", [128, 5, 128], BF16)
                    qf = T(st, "qf", [128, 8, 96], F32)
                    kvf = T(st, "kvf", [128, 8, 128], F32)
                    kf = T(st, "kf", [128, 8, 96], F32)
                    sqf = [T(st, f"sqf{i}", [128, 8, 96], F32) for i in range(2)]
                    r8 = [T(st, f"r8{i}", [128, 8], F32) for i in range(2)]
                    csA = T(st, "csA", [128, NT, 2, 16], F32)
                    cskA = T(st, "cskA", [128, NT, 2, 16], F32)
                    csiA = T(st, "csiA", [128, NT, 2, 16], I32)
                    rt = [T(st, f"rt{i}", [128, 4, 8, 16], F32) for i in range(2)]
                    qb = T(st, "qb", [128, 8, 96], BF16)
                    kb = T(st, "kb", [128, 8, 96], BF16)
                    vb = T(st, "vb", [128, 512], BF16)
                    sbq = T(st, "sbq", [128, 512], BF16)
                    sbk = T(st, "sbk", [128, 512], BF16)
                    sbv = T(st, "sbv", [128, 512], BF16)
                    gsb = T(st, "gsb", [128, 2048], BF16)
                    qTt = [T(st, f"qTt{i}", [128, 8, 128], BF16) for i in range(2)]
                    kTt = [T(st, f"kTt{i}", [128, 8, 128], BF16) for i in range(2)]
                    sqTt = [T(st, f"sqTt{i}", [128, 4, 128], BF16) for i in range(2)]
                    skTt = [T(st, f"skTt{i}", [128, 4, 128], BF16) for i in range(2)]
                    pT = [PS(st, f"pT{i}", [128, 1024], BF16) for i in range(2)]
                    pP = [PS(st, f"pP{i}", [128, 512]) for i in range(4)]
                    pqkv = PS(st, "pqkv", [128, 1024])

                    for kc in range(8):
                        dma('sp', lambda e: e.dma_start(out=win[:, kc, :], in_=WINB[kc * 128:(kc + 1) * 128, :]), writes=['win'])
                    for c in range(3):
                        dma('sp', lambda e: e.dma_start(out=wst[:, 0:768], in_=w_uq_d[c * 128:(c + 1) * 128, :]), writes=['wst'])
                        op('dve', lambda e: e.tensor_scalar(out=wuq[:, c, :], in0=wst[:, 0:768], scalar1=gvec[:, 16 + c:17 + c],
                                                            scalar2=None, op0=ALU.mult), reads=['wst', 'gvec'], writes=['wuq'])
                    for c in range(2):
                        dma('sp', lambda e: e.dma_start(out=wst[:], in_=w_ukv_d[c * 128:(c + 1) * 128, :]), writes=['wst'])
                        op('dve', lambda e: e.tensor_scalar(out=wukv[:, c, :], in0=wst[:], scalar1=gvec[:, 19 + c:20 + c],
                                                            scalar2=None, op0=ALU.mult), reads=['wst', 'gvec'], writes=['wukv'])
                    op('dve', lambda e: e.tensor_copy(out=shb[:], in_=modT[:, 0:8, b]), reads=['modT'], writes=['shb'])
                    groups = [(0, 512), (512, 160), (672, 512), (1184, 512), (1696, 512),
                              (2208, 512), (2720, 512), (3232, 512), (3744, 512)]
                    for gi, (c0, n) in enumerate(groups):
                        ps = pP[gi % 3]
                        for kc in range(8):
                            op('pe', lambda e: e.matmul(ps[0:1, 0:n], lhsT=shb[:, kc:kc + 1], rhs=win[:, kc, c0:c0 + n],
                                                        start=(kc == 0), stop=(kc == 7)), reads=['shb', 'win'], writes=[f'pP{gi % 3}'])
                        op('act', lambda e: e.activation(out=cb[0:1, c0:c0 + n], in_=ps[0:1, 0:n], func=AF.Copy), reads=[f'pP{gi % 3}'], writes=['cb'])
                    for kc in range(8):
                        op('dve', lambda e: e.tensor_scalar(out=win[:, kc, :], in0=win[:, kc, :], scalar1=a1T[:, kc, b:b + 1], scalar2=None, op0=ALU.mult),
                           reads=['win', 'a1T', 'cb'], writes=['win'])
                    TWO_PI = float(2 * np.pi)
                    op('dve', lambda e: e.tensor_tensor(out=csA[:, :, 1, :], in0=posf[:, b, :].unsqueeze(2).to_broadcast([128, NT, 16]),
                                                        in1=cst[:, C_FREQ:C_FREQ + 16].unsqueeze(1).to_broadcast([128, NT, 16]), op=ALU.mult),
                       reads=['posf', 'cst'], writes=['csA'])
                    op('dve', lambda e: e.tensor_scalar(out=csA[:, :, 0, :], in0=csA[:, :, 1, :], scalar1=float(0.5 * np.pi), scalar2=None, op0=ALU.add),
                       reads=['csA'], writes=['csA'])
                    op('dve', lambda e: e.tensor_scalar(out=cskA[:], in0=csA[:], scalar1=1.0 / TWO_PI, scalar2=None, op0=ALU.mult),
                       reads=['csA'], writes=['cskA'])
                    op('dve', lambda e: e.tensor_copy(out=csiA[:], in_=cskA[:]), reads=['cskA'], writes=['csiA'])
                    op('dve', lambda e: e.tensor_copy(out=cskA[:], in_=csiA[:]), reads=['csiA'], writes=['cskA'])
                    op('dve', lambda e: e.scalar_tensor_tensor(out=csA[:], in0=cskA[:], scalar=-TWO_PI, in1=csA[:], op0=ALU.mult, op1=ALU.add),
                       reads=['cskA', 'csA'], writes=['csA'])
                    op('act', lambda e: e.activation(out=csA[:], in_=csA[:], func=AF.Sin), reads=['csA'], writes=['csA'])
                    ck('c1')
                    tcur = [0]

                    def qk_chain(src, gsel, dstb, extra_scale, nm, w):
                        SQ, R8, RT = sqf[w], r8[w], rt[w]
                        sk_, rk_, tk_ = f'sqf{w}', f'r8{w}', f'rt{w}'
                        op('dve', lambda e: e.tensor_tensor(out=SQ[:], in0=src[:], in1=src[:], op=ALU.mult), reads=[nm], writes=[sk_])
                        op('dve', lambda e: e.tensor_reduce(out=R8[:], in_=SQ[:], axis=AX.X, op=ALU.add), reads=[sk_], writes=[rk_])
                        rstd_chain(R8[:], rk_, 1.0 / 96)
                        if extra_scale != 1.0:
                            op('dve', lambda e: e.tensor_scalar(out=R8[:], in0=R8[:], scalar1=extra_scale, scalar2=None, op0=ALU.mult),
                               reads=[rk_], writes=[rk_])
                        op('dve', lambda e: e.tensor_tensor(out=SQ[:], in0=src[:], in1=R8[:].unsqueeze(2).to_broadcast([128, 8, 96]),
                                                            op=ALU.mult), reads=[nm, rk_], writes=[sk_])
                        op('dve', lambda e: e.tensor_tensor(out=SQ[:], in0=SQ[:], in1=gqk[:, gsel, :].unsqueeze(1).to_broadcast([128, 8, 96]),
                                                            op=ALU.mult), reads=[sk_, 'gqk'], writes=[sk_])
                        x1 = SQ[:, :, 64:80]
                        x2 = SQ[:, :, 80:96]
                        cosb = csA[:, tcur[0], 0, :].unsqueeze(1).to_broadcast([128, 8, 16])
                        sinb = csA[:, tcur[0], 1, :].unsqueeze(1).to_broadcast([128, 8, 16])
                        op('dve', lambda e: e.tensor_tensor(out=RT[:, 0], in0=x1, in1=cosb, op=ALU.mult), reads=[sk_, 'csA'], writes=[tk_ + 'a'])
                        op('dve', lambda e: e.tensor_tensor(out=RT[:, 1], in0=x2, in1=sinb, op=ALU.mult), reads=[sk_, 'csA'], writes=[tk_ + 'b'])
                        op('pool', lambda e: e.tensor_tensor(out=RT[:, 2], in0=x2, in1=cosb, op=ALU.mult), reads=[sk_, 'csA'], writes=[tk_ + 'c'])
                        op('pool', lambda e: e.tensor_tensor(out=RT[:, 3], in0=x1, in1=sinb, op=ALU.mult), reads=[sk_, 'csA'], writes=[tk_ + 'd'])
                        op('dve', lambda e: e.tensor_tensor(out=dstb[:, :, 64:80], in0=RT[:, 0], in1=RT[:, 1], op=ALU.subtract),
                           reads=[tk_ + 'a', tk_ + 'b'], writes=[nm + 'b'])
                        op('pool', lambda e: e.tensor_tensor(out=dstb[:, :, 80:96], in0=RT[:, 2], in1=RT[:, 3], op=ALU.add),
                           reads=[tk_ + 'c', tk_ + 'd'], writes=[nm + 'b'])
                        op('act', lambda e: e.activation(out=dstb[:, :, 0:64], in_=SQ[:, :, 0:64], func=AF.Copy), reads=[sk_], writes=[nm + 'b'])

                    TWO_PI = float(2 * np.pi)
                    pcnt = [0]
                    tcnt = [0]

                    def xload(t):
                        n = t % 2
                        r0 = b * S + t * 128
                        dma('sp', lambda e: e.dma_start(out=xt[n][:], in_=x_d[r0:r0 + 128, :]), writes=[f'xt{n}'])

                    def front(t):
                        n = t % 2
                        X, xk = xt[n], f'xt{n}'
                        op('act', lambda e: e.activation(out=junk[:], in_=X[:], func=AF.Square, accum_out=ss[:, 0:1]), reads=[xk], writes=['ss0'])
                        rstd_chain(ss[:, 0:1], 'ss0', 1.0 / D)
                        op('act', lambda e: e.activation(out=hb[n][:], in_=X[:], func=AF.Copy, scale=ss[:, 0:1]), reads=[xk, 'ss0'], writes=[f'hb{n}'])
                        pt = tcnt[0] % 2
                        tcnt[0] += 1
                        for c in range(8):
                            op('pe', lambda e: e.transpose(out=pT[pt][:, c * 128:(c + 1) * 128], in_=hb[n][:, c * 128:(c + 1) * 128], identity=identb),
                               reads=[f'hb{n}', 'cbf'], writes=[f'pT{pt}'], inc=(c == 7))
                        op('act', lambda e: e.activation(out=hT[n][:].rearrange("p c t -> p (c t)"), in_=pT[pt][:], func=AF.Copy),
                           reads=[f'pT{pt}'], writes=[f'hT{n}'])

                    xload(0)
                    front(0)
                    xload(1)
                    for t in range(NT):
                        n = t % 2
                        tcur[0] = t
                        HT, hk = hT[n], f'hT{n}'
                        conv_step(2 if nseq >= 4 else 6)

                        def proj(c0, n_):
                            pi = pcnt[0] % 4
                            pcnt[0] += 1
                            ps = pP[pi]
                            for kc in range(8):
                                op('pe', lambda e: e.matmul(ps[:, 0:n_], lhsT=HT[:, kc, :], rhs=win[:, kc, c0:c0 + n_],
                                                            start=(kc == 0), stop=False), reads=[hk, 'win'], writes=[f'pP{pi}'], inc=False)
                            op('pe', lambda e: e.matmul(ps[:, 0:n_], lhsT=onesb[0:1, :], rhs=cb[0:1, c0:c0 + n_], start=False, stop=True),
                               reads=['cbf', 'cb'], writes=[f'pP{pi}'])
                            return ps, f'pP{pi}'

                        def transposes(srcs, width=128):
                            pt = tcnt[0] % 2
                            tcnt[0] += 1
                            for ci, (ap_, key_) in enumerate(srcs):
                                rows = ap_.shape[-1]
                                op('pe', lambda e: e.transpose(out=pT[pt][0:rows, ci * 128:(ci + 1) * 128], in_=ap_, identity=identb),
                                   reads=[key_, 'cbf'], writes=[f'pT{pt}'], inc=(ci == len(srcs) - 1))
                            return pT[pt], f'pT{pt}'

                        psA, kA = proj(0, 512)
                        psB, kB = proj(512, 160)
                        op('act', lambda e: e.activation(out=lat[:, 0:512], in_=psA[:], func=AF.Copy), reads=[kA], writes=['lat'])
                        op('dve', lambda e: e.tensor_copy(out=lat[:, 512:672], in_=psB[:, 0:160]), reads=[kB], writes=['lat'])
                        psQ, kQ = proj(672, 512)
                        psK, kK = proj(1184, 512)
                        op('act', lambda e: e.activation(out=junk[:, 0:384], in_=lat[:, 0:384], func=AF.Square, accum_out=ss[:, 1:2]),
                           reads=['lat'], writes=['ss12'])
                        op('act', lambda e: e.activation(out=junk[:, 0:256], in_=lat[:, 384:640], func=AF.Square, accum_out=ss[:, 2:3]),
                           reads=['lat'], writes=['ss12'])
                        op('dve', lambda e: e.tensor_scalar(out=ss[:, 1:2], in0=ss[:, 1:2], scalar1=256.0 / 384.0, scalar2=None, op0=ALU.mult),
                           reads=['ss12'], writes=['ss12'])
                        rstd_chain(ss[:, 1:3], 'ss12', 1.0 / 256)
                        op('dve', lambda e: e.tensor_scalar(out=latb[:, 0:384], in0=lat[:, 0:384], scalar1=ss[:, 1:2], scalar2=None, op0=ALU.mult),
                           reads=['lat', 'ss12'], writes=['latb'])
                        op('dve', lambda e: e.tensor_scalar(out=latb[:, 384:640], in0=lat[:, 384:640], scalar1=ss[:, 2:3], scalar2=None, op0=ALU.mult),
                           reads=['lat', 'ss12'], writes=['latb'])
                        op('dve', lambda e: e.tensor_scalar(out=sbq[:], in0=psQ[:], scalar1=0.125, scalar2=None, op0=ALU.mult), reads=[kQ], writes=['sbq'])
                        op('act', lambda e: e.activation(out=sbk[:], in_=psK[:], func=AF.Copy), reads=[kK], writes=['sbk'])
                        ptl, ktl = transposes([(latb[:, c * 128:(c + 1) * 128], 'latb') for c in range(5)])
                        op('act', lambda e: e.activation(out=latT[:].rearrange("p c t -> p (c t)"), in_=ptl[:, 0:640], func=AF.Copy),
                           reads=[ktl], writes=['latT'])
                        for (c0, n_) in ((0, 512), (512, 256)):
                            for c in range(3):
                                op('pe', lambda e: e.matmul(pqkv[:, c0:c0 + n_], lhsT=latT[:, c, :], rhs=wuq[:, c, c0:c0 + n_],
                                                            start=(c == 0), stop=(c == 2)), reads=['latT', 'wuq'], writes=['pqkv'],
                                   inc=(c == 2 and c0 == 512))
                        op('act', lambda e: e.activation(out=qf[:].rearrange("p h d -> p (h d)"), in_=pqkv[:, 0:768], func=AF.Copy),
                           reads=['pqkv'], writes=['qf'])
                        psV, kV = proj(1696, 512)
                        for c0 in (0, 512):
                            for c in range(2):
                                op('pe', lambda e: e.matmul(pqkv[:, c0:c0 + 512], lhsT=latT[:, 3 + c, :], rhs=wukv[:, c, c0:c0 + 512],
                                                            start=(c == 0), stop=(c == 1)), reads=['latT', 'wukv'], writes=['pqkv'],
                                   inc=(c == 1 and c0 == 512))
                        op('dve', lambda e: e.tensor_copy(out=kvf[:].rearrange("p h d -> p (h d)"), in_=pqkv[:]), reads=['pqkv'], writes=['kvf'])
                        op('act', lambda e: e.activation(out=sbv[:], in_=psV[:], func=AF.Copy), reads=[kV], writes=['sbv'])
                        dma('sp', lambda e: e.dma_start(out=SV[b, t * 128:(t + 1) * 128, :], in_=sbv[:]), reads=['sbv'], writes=[('SV', b, t)])
                        qk_chain(qf, 0, qb, 96 ** -0.5, 'qf', 0)
                        pts, kts = transposes([(sbq[:, c * 128:(c + 1) * 128], 'sbq') for c in range(4)] +
                                              [(sbk[:, c * 128:(c + 1) * 128], 'sbk') for c in range(4)])
                        op('act', lambda e: e.activation(out=sqTt[n][:].rearrange("p c t -> p (c t)"), in_=pts[:, 0:512], func=AF.Copy),
                           reads=[kts], writes=[f'sqTt{n}'])
                        op('act', lambda e: e.activation(out=skTt[n][:].rearrange("p c t -> p (c t)"), in_=pts[:, 512:1024], func=AF.Copy),
                           reads=[kts], writes=[f'skTt{n}'])
                        dma('sp', lambda e: e.dma_start(out=SQT[b].rearrange("(c p) t -> p c t", p=128)[:, :, t * 128:(t + 1) * 128], in_=sqTt[n][:]),
                            reads=[f'sqTt{n}'], writes=[('SQT', b, t)])
                        dma('sp', lambda e: e.dma_start(out=SKT[b].rearrange("(c p) t -> p c t", p=128)[:, :, t * 128:(t + 1) * 128], in_=skTt[n][:]),
                            reads=[f'skTt{n}'], writes=[('SKT', b, t)])
                        op('pool', lambda e: e.tensor_copy(out=vb[:].rearrange("p (h d) -> p h d", d=64), in_=kvf[:, :, 64:128]),
                           reads=['kvf'], writes=['vb'])
                        dma('sp', lambda e: e.dma_start(out=VV[b, t * 128:(t + 1) * 128, :], in_=vb[:]), reads=['vb'], writes=[('VV', b, t)])
                        op('pool', lambda e: e.tensor_copy(out=kf[:, :, 0:64], in_=kvf[:, :, 0:64]), reads=['kvf'], writes=['kf'])
                        op('pool', lambda e: e.tensor_copy(out=kf[:, :, 64:96], in_=lat[:, 640:672].unsqueeze(1).to_broadcast([128, 8, 32])),
                           reads=['lat'], writes=['kf'])
                        for g in range(4):
                            psG, kG = proj(2208 + g * 512, 512)
                            op('act', lambda e: e.activation(out=gsb[:, g * 512:(g + 1) * 512], in_=psG[:], func=AF.Sigmoid), reads=[kG], writes=['gsb'])
                        dma('sp', lambda e: e.dma_start(out=GS[b, t * 128:(t + 1) * 128, :], in_=gsb[:]), reads=['gsb'], writes=[('GS', b, t)])
                        if t + 1 < NT:
                            front(t + 1)
                        if t + 2 < NT:
                            xload(t + 2)
                        qk_chain(kf, 1, kb, 1.0, 'kf', 1)
                        ptq, ktq = transposes([(qb[:, h, :], 'qfb') for h in range(H)])
                        op('act', lambda e: e.activation(out=qTt[n][0:96, :, :], in_=ptq[0:96, :].rearrange("p (h t) -> p h t", t=128), func=AF.Copy),
                           reads=[ktq], writes=[f'qTt{n}'])
                        dma('sp', lambda e: e.dma_start(out=QT[b].rearrange("h d t -> d h t")[:, :, t * 128:(t + 1) * 128], in_=qTt[n][0:96, :, :]),
                            reads=[f'qTt{n}'], writes=[('QT', b, t)])
                        ptk, ktk = transposes([(kb[:, h, :], 'kfb') for h in range(H)])
                        op('act', lambda e: e.activation(out=kTt[n][0:96, :, :], in_=ptk[0:96, :].rearrange("p (h t) -> p h t", t=128), func=AF.Copy),
                           reads=[ktk], writes=[f'kTt{n}'])
                        dma('sp', lambda e: e.dma_start(out=KT[b].rearrange("h d t -> d h t")[:, :, t * 128:(t + 1) * 128], in_=kTt[n][0:96, :, :]),
                            reads=[f'kTt{n}'], writes=[('KT', b, t)])
                    Sc.barrier()
                    if upto == 'P1':
                        return nc

                with ExitStack() as st:
                    st.enter_context(nc.named_scope(f'P2_{b}'))
                    qh = [T(st, f"qh{i}", [128, S], BF16) for i in range(2)]
                    kh = [T(st, f"kh{i}", [128, S], BF16) for i in range(2)]
                    vaug = T(st, "vaug", [128, NT, H, 128], BF16)
                    NPB = 6
                    pb = [T(st, f"pb{i}", [128, 512], BF16) for i in range(NPB)]
                    rden = [T(st, f"rden{i}", [128, 512], F32) for i in range(2)]
                    ost = [T(st, f"ost{i}", [64, 512], BF16) for i in range(2)]
                    pS = [PS(st, f"pS{i}", [128, 512]) for i in range(6)]
                    pO = [PS(st, f"pO{i}", [128, 512]) for i in range(2)]
                    op('pool', lambda e: e.memset(vaug[:, :, :, 64:128], 1.0), writes=['vall'])
                    for t_ in range(NT):
                        dma('sp', lambda e: e.dma_start(out=vaug[:, t_, :, 0:64], in_=VV[b, t_ * 128:(t_ + 1) * 128, :].rearrange("p (h d) -> p h d", d=64)),
                            reads=[('VV', b, t_)], writes=['vall'])

                    def load_head(h):
                        dma('sp', lambda e: e.dma_start(out=qh[h % 2][0:96, :], in_=QT[b, h]), reads=[('QT', b)], writes=[f'qh{h % 2}'])
                        dma('sp', lambda e: e.dma_start(out=kh[h % 2][0:96, :], in_=KT[b, h]), reads=[('KT', b)], writes=[f'kh{h % 2}'])

                    items = [(h, Q, kt) for h in range(H) for Q in range(4) for kt in range(4 * Q + 4)]

                    def geom(idx):
                        h, Q, kt = items[idx]
                        diag = kt >= 4 * Q
                        c0 = 128 * (kt - 4 * Q) if diag else 0
                        return h, Q, kt, diag, c0

                    def stA(idx):
                        h, Q, kt, diag, c0 = geom(idx)
                        if Q == 0 and kt == 0:
                            if h == 0:
                                load_head(0)
                            if h + 1 < H:
                                load_head(h + 1)
                        Qh, Kh = qh[h % 2], kh[h % 2]
                        psn, pbn = idx % 6, idx % NPB
                        op('pe', lambda e: e.matmul(pS[psn][:, c0:512], lhsT=Kh[0:96, kt * 128:(kt + 1) * 128],
                                                    rhs=Qh[0:96, Q * 512 + c0:(Q + 1) * 512], start=True, stop=True),
                           reads=[f'qh{h % 2}', f'kh{h % 2}'], writes=[f'pS{psn}'])
                        op('act', lambda e: e.activation(out=pb[pbn][:, c0:512], in_=pS[psn][:, c0:512], func=AF.Exp),
                           reads=[f'pS{psn}'], writes=[f'pb{pbn}'])
                        if diag:
                            op('pool', lambda e: e.memset(pb[pbn][64:128, c0:c0 + 64], 0.0), reads=[f'pb{pbn}'], writes=[f'pb{pbn}'])

                    def stB(idx):
                        h, Q, kt, diag, c0 = geom(idx)
                        pbn = idx % NPB
                        g = (h * 4 + Q) % 2
                        nk = 4 * Q + 4
                        op('pe', lambda e: e.matmul(pO[g][:, c0:512], lhsT=vaug[:, kt, h, :], rhs=pb[pbn][:, c0:512],
                                                    start=(kt == 0), stop=(kt == nk - 1)),
                           reads=['vall', f'pb{pbn}'], writes=[f'pO{g}'])
                        if kt == nk - 1:
                            op('dve', lambda e: e.reciprocal(out=rden[g][64:128, :], in_=pO[g][64:128, :]), reads=[f'pO{g}'], writes=[f'rden{g}'])
                            op('dve', lambda e: e.tensor_tensor(out=ost[g][:], in0=pO[g][0:64, :], in1=rden[g][64:128, :], op=ALU.mult),
                               reads=[f'pO{g}', f'rden{g}'], writes=[f'ost{g}'])
                            dma('sp', lambda e: e.dma_start(out=OM[b, h * 64:(h + 1) * 64, Q * 512:(Q + 1) * 512], in_=ost[g][:]),
                                reads=[f'ost{g}'], writes=[('OM', b, h, Q)])

                    LA = 3
                    for idx in range(len(items) + LA):
                        if idx < len(items):
                            stA(idx)
                        if idx >= LA:
                            stB(idx - LA)
                    Sc.barrier()
                    if upto == 'P2':
                        return nc

                with ExitStack() as st:
                    st.enter_context(nc.named_scope(f'P3_{b}'))
                    sq2 = [T(st, f"sq2{i}", [128, S], BF16) for i in range(2)]
                    sk2 = [T(st, f"sk2{i}", [128, 2, S], BF16) for i in range(2)]
                    svall = T(st, "svall", [128, NT, 512], BF16)
                    NZ, NSPF, NSPB, NAB, NCAR = 4, 6, 4, 4, 3
                    ef = [T(st, f"ef{i}", [128, 512], F32) for i in range(2)]
                    spb = [T(st, f"spb{i}", [128, 512], BF16) for i in range(NSPB)]
                    la2 = [T(st, f"la2{i}", [128, 512], F32) for i in range(2)]
                    ab = [T(st, f"ab{i}", [128, 512], BF16) for i in range(NAB)]
                    car = [T(st, f"car{i}", [128, 512], F32) for i in range(NCAR)]
                    osb = [T(st, f"osb{i}", [128, 512], BF16) for i in range(2)]
                    pZ = [PS(st, f"pZ{i}", [128, 512]) for i in range(NZ)]
                    pC = [PS(st, f"pC{i}", [128, 512]) for i in range(2)]
                    pO = [PS(st, f"pO3{i}", [128, 512]) for i in range(2)]
                    dma('sp', lambda e: e.dma_start(out=svall[:], in_=SV[b].rearrange("(t p) c -> p t c", p=128)),
                        reads=[('SV', b, t) for t in range(NT)], writes=['svall'])

                    def load_pair(c):
                        dma('sp', lambda e: e.dma_start(out=sq2[c % 2][:], in_=SQT[b, c * 128:(c + 1) * 128, :]), reads=[('SQT', b)], writes=[f'sq2{c % 2}'])
                        for hp_ in range(2):
                            dma('sp', lambda e: e.dma_start(out=sk2[c % 2][:, hp_, :], in_=SKT[b, c * 128:(c + 1) * 128, :]), reads=[('SKT', b)], writes=[f'sk2{c % 2}'])
                            z0 = 64 * (1 - hp_)
                            op('pool', lambda e: e.memset(sk2[c % 2][z0:z0 + 64, hp_, :], 0.0), reads=[f'sk2{c % 2}'], writes=[f'sk2{c % 2}'])

                    items = []
                    for c in range(4):
                        for hp in range(2):
                            for Q in range(4):
                                for kt in range(4 * Q + 3, -1, -1):
                                    items.append((c, hp, Q, kt))
                    NI = len(items)

                    def geom(idx):
                        c, hp, Q, kt = items[idx]
                        diag = kt >= 4 * Q
                        c0 = 128 * (kt - 4 * Q) if diag else 0
                        first = (kt == 4 * Q + 3)
                        last = (kt == 0)
                        grp = (c * 2 + hp) * 4 + Q
                        return c, hp, Q, kt, diag, c0, first, last, grp

                    def stA(idx):
                        c, hp, Q, kt, diag, c0, first, last, grp = geom(idx)
                        if hp == 0 and Q == 0 and first:
                            if c == 0:
                                load_pair(0)
                            if c + 1 < 4:
                                load_pair(c + 1)
                        p0 = 64 * hp
                        zs, es, fs, bs = idx % NZ, idx % 2, idx % NSPF, idx % NSPB
                        op('pe', lambda e: e.matmul(pZ[zs][:, c0:512], lhsT=sk2[c % 2][:, hp, kt * 128:(kt + 1) * 128],
                                                    rhs=sq2[c % 2][:, Q * 512 + c0:(Q + 1) * 512], start=True, stop=False,
                                                    skip_group_check=True),
                           reads=[f'sq2{c % 2}', f'sk2{c % 2}'], writes=[f'pZ{zs}'])
                        op('act', lambda e: e.activation(out=ef[es][:, c0:512], in_=pZ[zs][:, c0:512], func=AF.Exp),
                           reads=[f'pZ{zs}'], writes=[f'ef{es}'])
                        op('act', lambda e: e.activation(out=spb[bs][:, c0:512], in_=ef[es][:, c0:512], func=AF.Ln, bias=1.0),
                           reads=[f'ef{es}'], writes=[f'spb{bs}'])
                        if diag:
                            op('pool', lambda e: e.tensor_tensor(out=spb[bs][:, c0:c0 + 128], in0=spb[bs][:, c0:c0 + 128], in1=Rb, op=ALU.mult),
                               reads=[f'spb{bs}', 'cbf'], writes=[f'spb{bs}'])

                    def stB(idx):
                        c, hp, Q, kt, diag, c0, first, last, grp = geom(idx)
                        zs, bs, cs_ = idx % NZ, idx % NSPB, idx % 2
                        op('pe', lambda e: e.matmul(pZ[zs][:, c0:512], lhsT=nub[:], rhs=spb[bs][:, c0:512], start=False, stop=True,
                                                    skip_group_check=True),
                           reads=['cbf', f'spb{bs}', f'ef{idx % 2}'], writes=[f'pZ{zs}'], inc=False)
                        op('pe', lambda e: e.matmul(pC[cs_][:, c0:512], lhsT=onesb, rhs=spb[bs][:, c0:512], start=True, stop=True),
                           reads=['cbf', f'spb{bs}'], writes=[f'pC{cs_}'])

                    kstep = [0]
                    csrc = {}

                    def stC(idx):
                        c, hp, Q, kt, diag, c0, first, last, grp = geom(idx)
                        zs, fs, cs_, ls, as_ = idx % NZ, idx % NSPF, idx % 2, idx % 2, idx % NAB
                        if first:
                            kstep[0] = 0
                            for ci in range(NCAR):
                                op('pool', lambda e: e.memset(car[ci][:], 0.0), writes=[f'car{ci}'])
                        k = kstep[0]
                        kstep[0] += 1
                        cin, cout = k % NCAR, (k + 1) % NCAR
                        if first:
                            src = pZ[zs]
                            srck = f'pZ{zs}'
                        else:
                            op('dve', lambda e: e.tensor_tensor(out=la2[ls][:, c0:512], in0=pZ[zs][:, c0:512], in1=car[cin][:, c0:512], op=ALU.subtract),
                               reads=[f'pZ{zs}', f'car{cin}'], writes=[f'la2{ls}'])
                            src = la2[ls]
                            srck = f'la2{ls}'
                        if not last:
                            op('dve', lambda e: e.tensor_tensor(out=car[cout][:, c0:512], in0=car[cin][:, c0:512], in1=pC[cs_][:, c0:512], op=ALU.add),
                               reads=[f'car{cin}', f'pC{cs_}'], writes=[f'car{cout}'])
                        csrc[idx] = (src, srck)

                    def stC2(idx):
                        c, hp, Q, kt, diag, c0, first, last, grp = geom(idx)
                        as_ = idx % NAB
                        src, srck = csrc.pop(idx)
                        op('act', lambda e: e.activation(out=ab[as_][:, c0:512], in_=src[:, c0:512], func=AF.Exp),
                           reads=[srck], writes=[f'ab{as_}'])
                        if diag:
                            op('pool', lambda e: e.tensor_tensor(out=ab[as_][:, c0:c0 + 128], in0=ab[as_][:, c0:c0 + 128], in1=Rb, op=ALU.mult),
                               reads=[f'ab{as_}', 'cbf'], writes=[f'ab{as_}'])

                    def stD(idx):
                        c, hp, Q, kt, diag, c0, first, last, grp = geom(idx)
                        as_ = idx % NAB
                        h = 2 * c + hp
                        g = grp % 2
                        p0 = 64 * hp
                        op('pe', lambda e: e.matmul(pO[g][:, c0:512], lhsT=svall[:, kt, c * 128:(c + 1) * 128], rhs=ab[as_][:, c0:512],
                                                    start=first, stop=last, skip_group_check=True),
                           reads=['svall', f'ab{as_}'], writes=[f'pO3{g}'])
                        if last:
                            op('act', lambda e: e.activation(out=osb[g][p0:p0 + 64, :], in_=pO[g][p0:p0 + 64, :], func=AF.Copy), reads=[f'pO3{g}'], writes=[f'osb{g}'])
                            dma('sp', lambda e: e.dma_start(out=OS[b, h * 64:(h + 1) * 64, Q * 512:(Q + 1) * 512], in_=osb[g][p0:p0 + 64, :]),
                                reads=[f'osb{g}'], writes=[('OS', b, h, Q)])

                    LB, LC, LD = 2, 3, 5
                    for step in range(NI + LD):
                        if 0 <= step - LC < NI:
                            stC(step - LC)
                        if step < NI:
                            stA(step)
                        if 0 <= step - LB < NI:
                            stB(step - LB)
                        if 0 <= step - LC < NI:
                            stC2(step - LC)
                        if 0 <= step - LD < NI:
                            stD(step - LD)
                    Sc.barrier()
                    if upto == 'P3':
                        return nc

                with ExitStack() as st:
                    st.enter_context(nc.named_scope(f'P4_{b}'))
                    wom = T(st, "wom", [128, 4, D], BF16)
                    wos = T(st, "wos", [128, 4, D], BF16)
                    wout = T(st, "wout", [128, 8, D], BF16)
                    wr = T(st, "wr", [128, 8, NE], F32)
                    omt = [T(st, f"omt{i}", [128, 4, 128], BF16) for i in range(2)]
                    ostt = [T(st, f"ostt{i}", [128, 4, 128], BF16) for i in range(2)]
                    gst = [T(st, f"gst{i}", [128, 2048], BF16) for i in range(2)]
                    xt = [T(st, f"x4t{i}", [128, D], F32) for i in range(2)]
                    m1 = T(st, "m1", [128, D], F32)
                    m2 = T(st, "m2", [128, D], F32)
                    mb = T(st, "mb", [128, D], BF16)
                    mT = T(st, "mT", [128, 8, 128], BF16)
                    x1t = [T(st, f"x1t{i}", [128, D], F32) for i in range(2)]
                    junk = T(st, "junk4", [128, D], BF16)
                    ss = T(st, "ss4", [128, 4], F32)
                    h2f = T(st, "h2f", [128, D], F32)
                    h2b = [T(st, f"h2b{i}", [128, D], BF16) for i in range(2)]
                    h2T = T(st, "h2T", [128, 8, 128], F32)
                    m8 = T(st, "m8", [128, 8], F32)
                    mk = T(st, "mk", [128, NE], BF16)
                    e4 = T(st, "e4", [128, 4], F32)
                    pOA = PS(st, "pOA", [128, 1024])
                    pOB = PS(st, "pOB", [128, 1024])
                    pT = PS(st, "pT4", [128, 1024], BF16)
                    pY = PS(st, "pY", [128, 1024])
                    pL = PS(st, "pL", [128, 512])
                    for kc in range(4):
                        dma('pool', lambda e: e.dma_start(out=wom[:, kc, :], in_=w_om_d[kc * 128:(kc + 1) * 128, :]), writes=['wom'])
                        dma('pool', lambda e: e.dma_start(out=wos[:, kc, :], in_=w_os_d[kc * 128:(kc + 1) * 128, :]), writes=['wos'])
                    for kc in range(8):
                        dma('pool', lambda e: e.dma_start(out=wout[:, kc, :], in_=w_out_d[kc * 128:(kc + 1) * 128, :]), writes=['wout'])
                    dma('sp', lambda e: e.dma_start(out=wr[:], in_=w_r_d.rearrange("(c p) n -> p c n", p=128)), writes=['wr'])
                    make_bc(0, lambda c: modT[:, 16 + c, b:b + 1], 'modT', pOA)
                    make_bc(1, lambda c: a2T[:, c, b:b + 1], 'a2T', pOA)
                    make_bc(2, lambda c: modT[:, 24 + c, b:b + 1], 'modT', pOA)
                    for kc in range(8):
                        op('dve', lambda e: e.tensor_tensor(out=wout[:, kc, :], in0=wout[:, kc, :], in1=bc[:, 0, :], op=ALU.mult),
                           reads=['wout', 'bc0'], writes=['wout'])
                    Sc.barrier()
                    def loads4(t):
                        n = t % 2
                        r0 = b * S + t * 128
                        dma('sp', lambda e: e.dma_start(out=omt[n][:], in_=OM[b].rearrange("(c p) t -> p c t", p=128)[:, :, t * 128:(t + 1) * 128]),
                            reads=[('OM', b)], writes=[f'omt{n}'])
                        dma('sp', lambda e: e.dma_start(out=ostt[n][:], in_=OS[b].rearrange("(c p) t -> p c t", p=128)[:, :, t * 128:(t + 1) * 128]),
                            reads=[('OS', b)], writes=[f'ostt{n}'])
                        dma('sp', lambda e: e.dma_start(out=gst[n][:], in_=GS[b, t * 128:(t + 1) * 128, :]), reads=[('GS', b, t)], writes=[f'gst{n}'])
                        dma('sp', lambda e: e.dma_start(out=xt[n][:], in_=x_d[r0:r0 + 128, :]), writes=[f'x4t{n}'])

                    h2f2 = [h2f, T(st, "h2fB", [128, D], F32)]

                    def H1a(t):
                        n = t % 2
                        for half in range(2):
                            hs = slice(half * 512, (half + 1) * 512)
                            for c in range(4):
                                op('pe', lambda e: e.matmul(pOA[:, hs], lhsT=omt[n][:, c, :], rhs=wom[:, c, hs], start=(c == 0), stop=(c == 3)),
                                   reads=[f'omt{n}', 'wom'], writes=['pbc'], inc=(c == 3))
                            for c in range(4):
                                op('pe', lambda e: e.matmul(pOB[:, hs], lhsT=ostt[n][:, c, :], rhs=wos[:, c, hs], start=(c == 0), stop=(c == 3)),
                                   reads=[f'ostt{n}', 'wos'], writes=['pOB'], inc=(c == 3))
                        op('dve', lambda e: e.tensor_tensor(out=m1[:], in0=pOA[:], in1=gst[n][:, 0:1024], op=ALU.mult),
                           reads=['pbc', f'gst{n}'], writes=['m1'])
                        op('dve', lambda e: e.tensor_tensor(out=m2[:], in0=pOB[:], in1=gst[n][:, 1024:2048], op=ALU.mult),
                           reads=['pOB', f'gst{n}'], writes=['m2'])
                        op('dve', lambda e: e.tensor_tensor(out=mb[:], in0=m1[:], in1=m2[:], op=ALU.add), reads=['m1', 'm2'], writes=['mb'])

                    def H1b(t):
                        n = t % 2
                        for c in range(8):
                            op('pe', lambda e: e.transpose(out=pT[:, c * 128:(c + 1) * 128], in_=mb[:, c * 128:(c + 1) * 128], identity=identb),
                               reads=['mb', 'cbf'], writes=['pT4'], inc=(c == 7))
                        op('act', lambda e: e.activation(out=mT[:].rearrange("p c t -> p (c t)"), in_=pT[:], func=AF.Copy), reads=['pT4'], writes=['mT'])
                        for half in range(2):
                            hs = slice(half * 512, (half + 1) * 512)
                            for c in range(8):
                                op('pe', lambda e: e.matmul(pY[:, hs], lhsT=mT[:, c, :], rhs=wout[:, c, hs], start=(c == 0), stop=(c == 7)),
                                   reads=['mT', 'wout'], writes=['pY'], inc=(c == 7))

                    def H1c(t):
                        i = b * NT + t
                        n = t % 2
                        r0 = b * S + t * 128
                        X1 = x1t[n]
                        x1k = f'x1t{n}'
                        HF, hfk = h2f2[n], f'h2f{n}'
                        op('dve', lambda e: e.tensor_tensor(out=X1[:], in0=pY[:], in1=xt[n][:], op=ALU.add), reads=['pY', f'x4t{n}'], writes=[x1k])
                        dma('sp', lambda e: e.dma_start(out=out_d[r0:r0 + 128, :], in_=X1[:]), reads=[x1k], writes=[('out', i)])
                        op('act', lambda e: e.activation(out=junk[:], in_=X1[:], func=AF.Square, accum_out=ss[:, 0:1]), reads=[x1k], writes=['ss4'])
                        rstd_chain(ss[:, 0:1], 'ss4', 1.0 / D)
                        op('dve', lambda e: e.scalar_tensor_tensor(out=m2[:], in0=X1[:], scalar=ss[:, 0:1], in1=bc[:, 1, :], op0=ALU.mult, op1=ALU.mult),
                           reads=[x1k, 'ss4', 'bc1'], writes=['m2'])
                        op('dve', lambda e: e.tensor_tensor(out=HF[:], in0=m2[:], in1=bc[:, 2, :], op=ALU.add), reads=['m2', 'bc2'], writes=[hfk])
                        op('act', lambda e: e.activation(out=h2b[n][:], in_=HF[:], func=AF.Copy), reads=[hfk], writes=[f'h2b{n}'])
                        dma('sp', lambda e: e.dma_start(out=H2[i * 128:(i + 1) * 128, :], in_=h2b[n][:]), reads=[f'h2b{n}'], writes=[('H2', i)])

                    def H2a(t):
                        n = t % 2
                        HF, hfk = h2f2[n], f'h2f{n}'
                        for c in range(8):
                            op('pe', lambda e: e.transpose(out=pOA[:, c * 128:(c + 1) * 128], in_=HF[:, c * 128:(c + 1) * 128], identity=ident),
                               reads=[hfk, 'cst'], writes=['pbc'], inc=(c == 7))
                        op('act', lambda e: e.activation(out=h2T[:].rearrange("p c t -> p (c t)"), in_=pOA[:], func=AF.Copy), reads=['pbc'], writes=['h2T'])
                        for c in range(8):
                            op('pe', lambda e: e.matmul(pL[:, 0:NE], lhsT=h2T[:, c, :], rhs=wr[:, c, :], start=(c == 0), stop=(c == 7)),
                               reads=['h2T', 'wr'], writes=['pL'], inc=(c == 7))

                    def H2b(t):
                        i = b * NT + t
                        op('dve', lambda e: e.tensor_tensor(out=LG[:, i, :], in0=pL[:, 0:NE], in1=brbc[:], op=ALU.add), reads=['pL', 'brbc'], writes=[('LG', i)])
                        op('dve', lambda e: e.max(out=V8[:, i, :], in_=LG[:, i, :]), reads=[('LG', i)], writes=[('V8', i)])
                        op('dve', lambda e: e.tensor_scalar(out=mk[:], in0=LG[:, i, :], scalar1=V8[:, i, 3:4], scalar2=None, op0=ALU.is_ge),
                           reads=[('LG', i), ('V8', i)], writes=['mk'])
                        op('pe', lambda e: e.matmul(pL[:, 32:64], lhsT=Rb, rhs=mk[:], start=True, stop=True, skip_group_check=True),
                           reads=['cbf', 'mk', ('LG', i)], writes=['pL2'], inc=False)
                        op('pe', lambda e: e.matmul(pL[:, 64:96], lhsT=onesb, rhs=mk[:], start=True, stop=True, skip_group_check=True),
                           reads=['cbf', 'mk'], writes=['pL2'])
                        op('dve', lambda e: e.tensor_tensor(out=POS[:, i, :], in0=pL[:, 32:64], in1=cum[:], op=ALU.add), reads=['pL2', 'cum'], writes=[('POS', i)])
                        op('dve', lambda e: e.tensor_tensor(out=cum[:], in0=pL[:, 64:96], in1=cum[:], op=ALU.add), reads=['pL2', 'cum'], writes=['cum', 'pLfree'])
                        op('dve', lambda e: e.tensor_scalar(out=m8[:, 0:1], in0=V8[:, i, 0:1], scalar1=-1.0, scalar2=None, op0=ALU.mult),
                           reads=[('V8', i)], writes=['m8'])
                        op('act', lambda e: e.activation(out=e4[:], in_=V8[:, i, 0:4], func=AF.Exp, bias=m8[:, 0:1], accum_out=m8[:, 1:2]),
                           reads=[('V8', i), 'm8'], writes=['e4', 'm8b'])
                        op('dve', lambda e: e.reciprocal(out=m8[:, 2:3], in_=m8[:, 1:2]), reads=['m8b'], writes=['m8c'])
                        op('dve', lambda e: e.tensor_scalar(out=W4[:, i, :], in0=e4[:], scalar1=m8[:, 2:3], scalar2=None, op0=ALU.mult),
                           reads=['e4', 'm8c'], writes=[('W4', i)])

                    loads4(0)
                    for t in range(NT + 1):
                        if t + 1 < NT:
                            loads4(t + 1)
                        if t < NT:
                            H1a(t)
                        if t >= 1:
                            H2a(t - 1)
                        if t < NT:
                            H1b(t)
                        if t >= 1:
                            H2b(t - 1)
                        if t < NT:
                            H1c(t)
                    Sc.barrier()
                    if upto == 'P4':
                        return nc

            with ExitStack() as st:
                p5scope = nc.named_scope('P5')
                p5scope.__enter__()
                conv_step(10 ** 6)
                cnt3 = T(st, "cnt3", [128, NE, NBM], F32)
                nblk = T(st, "nblk", [128, NE], F32)
                pend = T(st, "pend", [128, NE], F32)
                pstart = T(st, "pstart", [128, NE], F32)
                Dt = T(st, "Dt", [128, NE], F32)
                tmp = T(st, "tmp5", [128, NE], F32)
                idxf = T(st, "idxf", [128, 4], F32)
                h2l = [T(st, f"h2l{i}", [128, D], BF16) for i in range(6)]
                thr = cst[:, C_THR:C_THR + NBM]
                op('dve', lambda e: e.tensor_tensor(out=cnt3[:], in0=cum[:].unsqueeze(2).to_broadcast([128, NE, NBM]),
                                                    in1=thr.unsqueeze(1).to_broadcast([128, NE, NBM]), op=ALU.is_gt),
                   reads=['cum', 'cst'], writes=['cnt3'])
                op('dve', lambda e: e.tensor_reduce(out=nblk[:], in_=cnt3[:], axis=AX.X, op=ALU.add), reads=['cnt3'], writes=['nblk'])
                op('dve', lambda e: e.tensor_scalar(out=nblk[:], in0=nblk[:], scalar1=float(BLK), scalar2=None, op0=ALU.mult), reads=['nblk'], writes=['nblk'])
                op('dve', lambda e: e.tensor_copy(out=pend[:, 0:1], in_=nblk[:, 0:1]), reads=['nblk'], writes=['pend'])
                for ei in range(1, NE):
                    op('dve', lambda e: e.tensor_tensor(out=pend[:, ei:ei + 1], in0=pend[:, ei - 1:ei], in1=nblk[:, ei:ei + 1], op=ALU.add),
                       reads=['pend', 'nblk'], writes=['pend'])
                op('dve', lambda e: e.tensor_tensor(out=pstart[:], in0=pend[:], in1=nblk[:], op=ALU.subtract), reads=['pend', 'nblk'], writes=['pstart'])
                cnt3b = cnt3[:].rearrange("p e m -> p (e m)").rearrange("p (m e) -> p m e", e=NE)
                op('dve', lambda e: e.tensor_tensor(out=cnt3b, in0=pend[:].unsqueeze(1).to_broadcast([128, NBM, NE]),
                                                    in1=thr.unsqueeze(2).to_broadcast([128, NBM, NE]), op=ALU.is_le),
                   reads=['pend', 'cst', 'nblk'], writes=['cnt3'])
                op('dve', lambda e: e.tensor_reduce(out=BE[:], in_=cnt3b, axis=AX.X, op=ALU.add), reads=['cnt3'], writes=['BE'])
                op('dve', lambda e: e.tensor_scalar(out=BE[:], in0=BE[:], scalar1=float(NE - 1), scalar2=None, op0=ALU.min), reads=['BE'], writes=['BE'])
                for i in range(NTT):
                    n3 = i % 6
                    dma('sp', lambda e: e.dma_start(out=h2l[n3][:], in_=H2[i * 128:(i + 1) * 128, :]), reads=[('H2', i)], writes=[f'h2l{n3}'])
                    op('dve', lambda e: e.tensor_tensor(out=Dt[:], in0=POS[:, i, :], in1=pstart[:], op=ALU.add), reads=[('POS', i), 'pstart'], writes=['Dt'])
                    for k in range(4):
                        op('dve', lambda e: e.scalar_tensor_tensor(out=tmp[:], in0=LG[:, i, :], scalar=V8[:, i, k:k + 1], in1=Dt[:],
                                                                   op0=ALU.is_equal, op1=ALU.mult, accum_out=idxf[:, k:k + 1]),
                           reads=[('LG', i), ('V8', i), 'Dt'], writes=['tmp5', 'idxf'])
                    op('dve', lambda e: e.tensor_copy(out=DEST[:, i, :], in_=idxf[:]), reads=['idxf'], writes=[('DEST', i)])
                    for k in range(4):
                        dma('pool', lambda e: e.indirect_dma_start(out=XS, out_offset=bass.IndirectOffsetOnAxis(ap=DEST[:, i, k:k + 1], axis=0),
                                                                   in_=h2l[n3][:], in_offset=None),
                            reads=[('DEST', i), f'h2l{n3}'], writes=[('XS', i, k)])
                if dbg:
                    dma('sp', lambda e: e.dma_start(out=LGd, in_=LG[:].rearrange("p i e -> p (i e)")), reads=[('LG', i) for i in range(NTT)], writes=['LGd'])
                    dma('sp', lambda e: e.dma_start(out=DSTd, in_=DEST[:].rearrange("p i k -> p (i k)")), reads=[('DEST', i) for i in range(NTT)], writes=['DSTd'])
                    dma('sp', lambda e: e.dma_start(out=W4d, in_=W4[:].rearrange("p i k -> p (i k)")), reads=[('W4', i) for i in range(NTT)], writes=['W4d'])
                    dma('sp', lambda e: e.dma_start(out=BEd, in_=BE[:]), reads=['BE'], writes=['BEd'])
                Sc.barrier()
                if upto == 'P5':
                    return nc

                p5scope.__exit__(None, None, None)
            mid.close()

            with ExitStack() as s6:
                s6.enter_context(nc.named_scope('P6'))
                NJ = BLK // 128
                wgu = [T(s6, f"wgu{i}", [128, 8, 2 * D], BF16) for i in range(2)]
                wd = [T(s6, f"wd{i}", [128, 8, D], BF16) for i in range(2)]
                bguT = T(s6, "bguT", [128, 16, NE], F32)
                bdn = T(s6, "bdn", [NE, D], BF16)
                widxf = T(s6, "widxf", [128, NBM], F32)
                need = T(s6, "need", [128, NBM], F32)
                widx = T(s6, "widx", [128, NBM], I32)
                oh = [T(s6, f"oh{i}", [NE, 128], BF16) for i in range(2)]
                ohf = [T(s6, f"ohf{i}", [128, NE], F32) for i in range(2)]
                bprod = T(s6, "bprod", [128, 16, NE], F32)
                bsel = [T(s6, f"bsel{i}", [128, 16], F32) for i in range(2)]
                xl = [T(s6, f"xl{i}", [128, D], BF16) for i in range(3)]
                XT = [T(s6, f"XT{i}", [128, 8, BLK], BF16) for i in range(2)]
                hid = [T(s6, f"hid{i}", [128, 8, BLK], BF16) for i in range(2)]
                NEW = 4
                gg = [T(s6, f"gg{i}", [128, BLK], F32) for i in range(NEW)]
                sg = [T(s6, f"sg{i}", [128, BLK], F32) for i in range(NEW)]
                uu = [T(s6, f"uu{i}", [128, BLK], F32) for i in range(NEW)]
                yt = [T(s6, f"yt{i}", [128, D], F32) for i in range(2)]
                pT = [PS(s6, f"pT6{i}", [128, 1024], BF16) for i in range(2)]
                pG = [PS(s6, f"pG{i}", [128, 512]) for i in range(2)]
                pU = [PS(s6, f"pU{i}", [128, 512]) for i in range(2)]
                pY = PS(s6, "pY6", [128, 1024])
                dma('sp', lambda e: e.dma_start(out=bguT[:], in_=b_gu_d), writes=['bguT'])
                dma('pool', lambda e: e.dma_start(out=bdn[:], in_=b_d_d), writes=['bdn'])
                op('dve', lambda e: e.tensor_scalar(out=bguT[:, 8:16, :], in0=bguT[:, 8:16, :], scalar1=1.0, scalar2=None, op0=ALU.add),
                   reads=['bguT'], writes=['bguT'])
                HB = NB // 2
                BIG = 8192.0
                op('dve', lambda e: e.memset(need[:], 1.0), writes=['need'])
                op('dve', lambda e: e.tensor_tensor(out=need[:, 1:NB], in0=BE[:, 1:NB], in1=BE[:, 0:NB - 1], op=ALU.not_equal),
                   reads=['BE', 'need'], writes=['need'])
                op('dve', lambda e: e.memset(need[:, HB:HB + 1], 1.0), reads=['need'], writes=['need'])
                op('dve', lambda e: e.tensor_scalar(out=widxf[:], in0=BE[:], scalar1=128.0, scalar2=cst[:, C_IOTA:C_IOTA + 1],
                                                    op0=ALU.mult, op1=ALU.add), reads=['BE', 'cst'], writes=['widxf'])
                op('dve', lambda e: e.tensor_scalar(out=need[:], in0=need[:], scalar1=-BIG, scalar2=BIG, op0=ALU.mult, op1=ALU.add),
                   reads=['need'], writes=['need'])
                op('dve', lambda e: e.tensor_tensor(out=widxf[:], in0=widxf[:], in1=need[:], op=ALU.add), reads=['widxf', 'need'], writes=['widxf'])
                op('dve', lambda e: e.tensor_copy(out=widx[:], in_=widxf[:]), reads=['widxf'], writes=['widx'])
                LIM = 7.0
                order = []
                for i in range(HB):
                    order += [i, HB + i]
                bc_reg = nc.gpsimd.to_reg(NE * 128 - 1)

                def load_w(pos):
                    blk = order[pos]
                    n = pos % 2
                    dma('pool', lambda e: e.indirect_dma_start(out=wgu[n][:].rearrange("p kc c -> p (kc c)"), out_offset=None, in_=WGB,
                                                               in_offset=bass.IndirectOffsetOnAxis(ap=widx[:, blk:blk + 1], axis=0),
                                                               bounds_check=bc_reg, oob_is_err=False),
                        reads=['widx'], writes=[f'wgu{n}'])
                    dma('pool', lambda e: e.indirect_dma_start(out=wd[n][:].rearrange("p kc c -> p (kc c)"), out_offset=None, in_=WDB,
                                                               in_offset=bass.IndirectOffsetOnAxis(ap=widx[:, blk:blk + 1], axis=0),
                                                               bounds_check=bc_reg, oob_is_err=False),
                        reads=['widx'], writes=[f'wd{n}'])

                xcnt = [0]

                def prep_x(pos):
                    blk = order[pos]
                    n = pos % 2
                    op('dve', lambda e: e.tensor_scalar(out=oh[n][:], in0=BE[0:NE, blk:blk + 1].to_broadcast([NE, 128]),
                                                        scalar1=cst[0:NE, C_IOTA:C_IOTA + 1], scalar2=None, op0=ALU.is_equal),
                       reads=['BE', 'cst'], writes=[f'oh{n}'])
                    op('dve', lambda e: e.tensor_scalar(out=ohf[n][:], in0=cst[:, C_IOE:C_IOE + NE], scalar1=BE[:, blk:blk + 1], scalar2=None,
                                                        op0=ALU.is_equal), reads=['BE', 'cst'], writes=[f'ohf{n}'])
                    op('dve', lambda e: e.tensor_tensor(out=bprod[:], in0=bguT[:], in1=ohf[n][:].unsqueeze(1).to_broadcast([128, 16, NE]), op=ALU.mult),
                       reads=['bguT', f'ohf{n}'], writes=['bprod'])
                    op('dve', lambda e: e.tensor_reduce(out=bsel[n][:], in_=bprod[:], axis=AX.X, op=ALU.add), reads=['bprod'], writes=[f'bsel{n}'])
                    for j in range(NJ):
                        xn = xcnt[0] % 3
                        pn = xcnt[0] % 2
                        xcnt[0] += 1
                        r0 = blk * BLK + j * 128
                        dma('sp', lambda e: e.dma_start(out=xl[xn][:], in_=XS[r0:r0 + 128, :]), reads=['XS'], writes=[f'xl{xn}'])
                        for c in range(8):
                            op('pe', lambda e: e.transpose(out=pT[pn][:, c * 128:(c + 1) * 128], in_=xl[xn][:, c * 128:(c + 1) * 128], identity=identb),
                               reads=[f'xl{xn}', 'cbf'], writes=[f'pT6{pn}'], inc=(c == 7))
                        op('act', lambda e: e.activation(out=XT[n][:, :, j * 128:(j + 1) * 128], in_=pT[pn][:].rearrange("p (c t) -> p c t", t=128), func=AF.Copy),
                           reads=[f'pT6{pn}'], writes=[f'XT{n}'])

                load_w(0)
                prep_x(0)
                ecnt = 0
                ycnt = 0
                for pos in range(NB):
                    blk = order[pos]
                    n = pos % 2
                    if pos + 1 < NB:
                        load_w(pos + 1)
                    H_ = hid[n]
                    hk = f'hid{n}'
                    for fc in range(8):
                        pn = fc % 2
                        for (ps, pk, off) in ((pG[pn], f'pG{pn}', 0), (pU[pn], f'pU{pn}', D)):
                            for kc in range(8):
                                op('pe', lambda e: e.matmul(ps[:, 0:BLK], lhsT=wgu[n][:, kc, off + fc * 128:off + (fc + 1) * 128], rhs=XT[n][:, kc, :],
                                                            start=(kc == 0), stop=(kc == 7)), reads=[f'wgu{n}', f'XT{n}'], writes=[pk], inc=(kc == 7))
                        en = ecnt % NEW
                        ecnt += 1
                        G_, S_, U_ = gg[en], sg[en], uu[en]
                        op('dve', lambda e: e.tensor_scalar(out=G_[:], in0=pG[pn][:, 0:BLK], scalar1=bsel[n][:, fc:fc + 1], scalar2=LIM,
                                                            op0=ALU.add, op1=ALU.min), reads=[f'pG{pn}', f'bsel{n}'], writes=[f'gg{en}'])
                        op('act', lambda e: e.activation(out=S_[:], in_=G_[:], func=AF.Sigmoid, scale=1.702), reads=[f'gg{en}'], writes=[f'sg{en}'])
                        op('dve', lambda e: e.tensor_scalar(out=U_[:], in0=pU[pn][:, 0:BLK], scalar1=bsel[n][:, 8 + fc:9 + fc], scalar2=LIM + 1.0,
                                                            op0=ALU.add, op1=ALU.min), reads=[f'pU{pn}', f'bsel{n}'], writes=[f'uu{en}'])
                        op('dve', lambda e: e.tensor_tensor(out=S_[:], in0=G_[:], in1=S_[:], op=ALU.mult), reads=[f'gg{en}', f'sg{en}'], writes=[f'sg{en}'])
                        op('dve', lambda e: e.scalar_tensor_tensor(out=H_[:, fc, :], in0=U_[:], scalar=1.0 - LIM, in1=S_[:], op0=ALU.max, op1=ALU.mult),
                           reads=[f'uu{en}', f'sg{en}'], writes=[hk])
                        if fc == 6 and pos + 1 < NB:
                            prep_x(pos + 1)
                    for j in range(NJ):
                        yn = ycnt % 2
                        ycnt += 1
                        for half in range(2):
                            hs = slice(half * 512, (half + 1) * 512)
                            for fc in range(8):
                                op('pe', lambda e: e.matmul(pY[:, hs], lhsT=H_[:, fc, j * 128:(j + 1) * 128], rhs=wd[n][:, fc, hs],
                                                            start=(fc == 0), stop=False), reads=[hk, f'wd{n}'], writes=[f'pY6{half}'], inc=False)
                            op('pe', lambda e: e.matmul(pY[:, hs], lhsT=oh[n][:, 0:128], rhs=bdn[:, hs], start=False, stop=True),
                               reads=[f'oh{n}', 'bdn'], writes=[f'pY6{half}'])
                            op('act', lambda e: e.activation(out=yt[yn][:, hs], in_=pY[:, hs], func=AF.Copy), reads=[f'pY6{half}'], writes=[f'yt{yn}'])
                        r0 = blk * BLK + j * 128
                        dma('sp', lambda e: e.dma_start(out=YS[r0:r0 + 128, :], in_=yt[yn][:]), reads=[f'yt{yn}'], writes=[('YS', blk, j)])
                Sc.barrier()
                if upto == 'P6':
                    return nc

            with ExitStack() as st:
                st.enter_context(nc.named_scope('P7'))
                yk = [T(st, f"yk{i}", [128, D], F32) for i in range(8)]
                x1l = [T(st, f"x1l{i}", [128, D], F32) for i in range(2)]
                acc = [T(st, f"acc{i}", [128, D], F32) for i in range(2)]
                pbc7 = PS(st, "pbc7", [128, 1024])
                for i in range(NTT):
                    b, t = divmod(i, NT)
                    n = i % 2
                    if t == 0:
                        make_bc(0, lambda c: modT[:, 40 + c, b:b + 1], 'modT', pbc7)
                    dma('sp', lambda e: e.dma_start(out=x1l[n][:], in_=out_d[i * 128:(i + 1) * 128, :]), reads=[('out', i)], writes=[f'x1l{n}'])
                    for k in range(4):
                        yn = (i * 4 + k) % 8
                        dma('pool', lambda e: e.indirect_dma_start(out=yk[yn][:], out_offset=None, in_=YS,
                                                                   in_offset=bass.IndirectOffsetOnAxis(ap=DEST[:, i, k:k + 1], axis=0)),
                            reads=['YS', ('DEST', i)], writes=[f'yk{yn}'])
                    A = acc[n]
                    ak = f'acc{n}'
                    for k in range(4):
                        yn = (i * 4 + k) % 8
                        if k == 0:
                            op('dve', lambda e: e.tensor_scalar(out=A[:], in0=yk[yn][:], scalar1=W4[:, i, 0:1], scalar2=None, op0=ALU.mult),
                               reads=[f'yk{yn}', ('W4', i)], writes=[ak])
                        else:
                            op('dve', lambda e: e.scalar_tensor_tensor(out=A[:], in0=yk[yn][:], scalar=W4[:, i, k:k + 1], in1=A[:],
                                                                       op0=ALU.mult, op1=ALU.add), reads=[f'yk{yn}', ('W4', i), ak], writes=[ak])
                    op('dve', lambda e: e.tensor_tensor(out=A[:], in0=A[:], in1=bc[:, 0, :], op=ALU.mult), reads=[ak, 'bc0'], writes=[ak])
                    op('dve', lambda e: e.tensor_tensor(out=A[:], in0=A[:], in1=x1l[n][:], op=ALU.add), reads=[ak, f'x1l{n}'], writes=[ak])
                    dma('sp', lambda e: e.dma_start(out=out_d[i * 128:(i + 1) * 128, :], in_=A[:]), reads=[ak], writes=[('outf', i), ('out', i)])
                Sc.barrier()
            print("ops", Sc.nops, "waits", Sc.nwaits, flush=True)
    except _Stop:
        pass
    return nc


def host_inputs(inp, b0, nseq):
    f = np.float32
    x = np.ascontiguousarray(inp['x'][b0:b0 + nseq]).reshape(nseq * S, D).astype(f, copy=False)
    c = np.asarray(inp['c'][b0:b0 + nseq], f)
    cT = np.ascontiguousarray(c.reshape(nseq, 8, 128).transpose(2, 1, 0))
    pos = np.asarray(inp['positions'][b0:b0 + nseq], np.int32)
    posi = np.ascontiguousarray(pos.reshape(nseq, NT, 128).transpose(2, 0, 1))
    b_adaT = np.ascontiguousarray(np.asarray(inp['b_ada'][0], f).reshape(48, 128).T)
    gvec = np.concatenate([
        np.asarray(inp['g_norm1'][0], f).reshape(8, 128).T,
        np.asarray(inp['g_norm2'][0], f).reshape(8, 128).T,
        np.asarray(inp['g_q_lat'][0], f).reshape(3, 128).T,
        np.asarray(inp['g_kv_lat'][0], f).reshape(2, 128).T], axis=1)
    gqk = np.stack([np.asarray(inp['g_qk_q'][0], f), np.asarray(inp['g_qk_k'][0], f)], 0)
    gqk = np.ascontiguousarray(np.broadcast_to(gqk[None], (128, 2, 96)))
    return {
        'x': x, 'cT': cT, 'posi': posi,
        'w_ada': np.asarray(inp['w_ada'][0], f), 'b_adaT': b_adaT,
        'gvec': np.ascontiguousarray(gvec), 'gqk': gqk,
        'w_in': np.asarray(inp['w_in'][0], f), 'w_uq': np.asarray(inp['w_uq'][0], f),
        'w_ukv': np.asarray(inp['w_ukv'][0], f), 'w_o_mla': np.asarray(inp['w_o_mla'][0], f),
        'w_o_sb': np.asarray(inp['w_o_sb'][0], f), 'w_out': np.asarray(inp['w_out'][0], f),
        'w_router': np.asarray(inp['w_router'][0], f),
        'b_router_bc': np.ascontiguousarray(np.broadcast_to(np.asarray(inp['b_router'][0], f)[None], (128, NE))),
        'w_gu': np.asarray(inp['w_gate_up'][0], f).reshape(NE * D, 2 * D),
        'b_guT': np.ascontiguousarray(np.asarray(inp['b_gate_up'][0], f).reshape(NE, 16, 128).transpose(2, 1, 0)),
        'w_d': np.asarray(inp['w_down'][0], f).reshape(NE * D, D),
        'b_d': np.asarray(inp['b_down'][0], f),
        'consts': make_consts(),
    }


_NC_CACHE = {}


def kernel(**inputs):
    ncores, nseq = 8, 4
    if 'nc' not in _NC_CACHE:
        _NC_CACHE['nc'] = build(nseq)
    nc = _NC_CACHE['nc']
    in_maps = [host_inputs(inputs, core * nseq, nseq) for core in range(ncores)]
    res = run_bass_kernel_spmd(nc, in_maps, core_ids=list(range(ncores)))
    outs = [np.asarray(r['out']).reshape(nseq, S, D) for r in res.results]
    return np.concatenate(outs, axis=0).astype(np.float32)
```
